# Optimizing a Trainium2 kernel written in Bass

```python
import jax, jax.numpy as jnp
from jax import lax
import numpy as np

D_MODEL = 1024
BATCH = 2
SEQ = 8192
DEPTH = 1

D_MIX = D_MODEL
HG_WIDTH = D_MIX // 2
HG_DK = 128
HG_DV = 128
HG_HEADS = HG_WIDTH // HG_DV
HG_CHUNK = 64
NSA_WIDTH = D_MIX - HG_WIDTH
NSA_DH = 64
NSA_HEADS = NSA_WIDTH // NSA_DH
NSA_KV_HEADS = 2
NSA_GROUP = NSA_HEADS // NSA_KV_HEADS
CMP_LEN = 32
CMP_STRIDE = 16
SLC_LEN = 64
SLC_TOPK = 16
WIN = 512
Q_BLOCK = 128
PLE_DIM = 256
PEER_HEADS = 8
PEER_NKEYS = 128
PEER_N = PEER_NKEYS * PEER_NKEYS
PEER_DKEY = 256
PEER_TOPK = 16
PEER_TOKEN_BLOCK = 128
EPS = 1e-6

KV_WIDTH = NSA_KV_HEADS * NSA_DH
SPLIT_SIZES = (HG_WIDTH, HG_WIDTH, HG_WIDTH, HG_WIDTH, NSA_WIDTH,
               KV_WIDTH, KV_WIDTH, KV_WIDTH, KV_WIDTH, KV_WIDTH, KV_WIDTH, 3 * NSA_HEADS)
SPLIT_POINTS = tuple(int(v) for v in np.cumsum(SPLIT_SIZES)[:-1])
D_IN = int(sum(SPLIT_SIZES))

kernel_name = 'hybrid_hgrn2_nsa_peer_block'


def rmsnorm(x, g):
    x32 = x.astype(jnp.float32)
    y = x32 * lax.rsqrt(jnp.mean(x32 * x32, axis=-1, keepdims=True) + EPS)
    return (y * g.astype(jnp.float32)).astype(x.dtype)


def alibi_slopes(n_heads):
    return jnp.asarray(2.0 ** (-8.0 * np.arange(1, n_heads + 1) / n_heads), dtype=jnp.float32)


def masked_softmax(s, mask):
    s = jnp.where(mask, s, -jnp.inf)
    m = jnp.max(s, axis=-1, keepdims=True)
    m = jnp.where(jnp.isfinite(m), m, 0.0)
    e = jnp.where(mask, jnp.exp(s - m), 0.0)
    return e / jnp.maximum(jnp.sum(e, axis=-1, keepdims=True), 1e-30)


def hgrn2_mixer(q, f_logit, i_val, g, lb, out_gain):
    B, T, _ = q.shape
    f32 = jnp.float32
    C = HG_CHUNK
    n_chunk = T // C
    forget = lb + (1.0 - lb) * jax.nn.sigmoid(f_logit.astype(f32))
    log_f = jnp.log(forget)
    key = 1.0 - forget
    query = jax.nn.silu(q.astype(f32)) * HG_DK ** -0.5

    def chunks(a, d):
        return a.reshape(B, n_chunk, C, HG_HEADS, d).transpose(0, 3, 1, 2, 4)

    qc = chunks(query, HG_DK)
    kc = chunks(key, HG_DK)
    vc = chunks(i_val.astype(f32), HG_DV)
    G = jnp.cumsum(chunks(log_f, HG_DK), axis=3)
    G_ref = G[:, :, :, C // 2 - 1:C // 2]
    G_last = G[:, :, :, C - 1:C]
    causal = jnp.tril(jnp.ones((C, C), dtype=bool))
    a = jnp.einsum('bhntk,bhnsk->bhnts', qc * jnp.exp(G - G_ref), kc * jnp.exp(G_ref - G))
    o_intra = jnp.einsum('bhnts,bhnsv->bhntv', jnp.where(causal, a, 0.0), vc)
    q_in = jnp.moveaxis(qc * jnp.exp(G), 2, 0)
    k_in = jnp.moveaxis(kc * jnp.exp(G_last - G), 2, 0)
    v_in = jnp.moveaxis(vc, 2, 0)
    d_in = jnp.moveaxis(jnp.exp(G_last[:, :, :, 0]), 2, 0)

    def step(S, xs):
        q_t, k_t, v_t, d_t = xs
        o_t = jnp.einsum('bhtk,bhkv->bhtv', q_t, S)
        S = d_t[..., None] * S + jnp.einsum('bhtk,bhtv->bhkv', k_t, v_t)
        return S, o_t

    S0 = jnp.zeros((B, HG_HEADS, HG_DK, HG_DV), f32)
    _, o_inter = lax.scan(step, S0, (q_in, k_in, v_in, d_in))
    o = o_intra + jnp.moveaxis(o_inter, 0, 2)
    o = o.transpose(0, 2, 3, 1, 4).reshape(B, T, HG_HEADS, HG_DV)
    gate = jax.nn.silu(g.astype(f32)).reshape(B, T, HG_HEADS, HG_DV)
    return (rmsnorm(o, out_gain.reshape(HG_HEADS, HG_DV)) * gate).reshape(B, T, HG_WIDTH)


def cmp_to_slc(imp, n_slc):
    ratio = SLC_LEN // CMP_STRIDE
    span = CMP_LEN // CMP_STRIDE
    weights = np.convolve(np.ones(ratio), np.ones(span))
    n_cmp = imp.shape[-1]
    pad = n_slc * ratio + ratio + span - n_cmp
    padded = jnp.pad(imp, [(0, 0)] * (imp.ndim - 1) + [(0, pad)])
    out = float(weights[0]) * padded[..., 0:n_slc * ratio:ratio]
    for o in range(1, len(weights)):
        out = out + float(weights[o]) * padded[..., o:o + n_slc * ratio:ratio]
    return out


def nsa_mixer(q, k_cmp, v_cmp, k_slc, v_slc, k_win, v_win, gate_logit,
              q_gain, k_gain, cmp_pe, cmp_w1, cmp_w2, out_gain):
    B, T, _ = q.shape
    f32 = jnp.float32
    G, R = NSA_KV_HEADS, NSA_GROUP

    def heads(a, n):
        return a.astype(f32).reshape(B, T, n, NSA_DH).transpose(0, 2, 1, 3)

    qh = rmsnorm(heads(q, NSA_HEADS), q_gain) * NSA_DH ** -0.5
    qg = qh.reshape(B, G, R, T, NSA_DH)
    n_cmp = (T - CMP_LEN) // CMP_STRIDE + 1

    def compress(a, pe, w1, w2):
        c = a.reshape(B, G, T // CMP_STRIDE, CMP_STRIDE, NSA_DH)
        blk = jnp.concatenate([c[:, :, j:j + n_cmp] for j in range(CMP_LEN // CMP_STRIDE)], axis=3)
        blk = (blk + pe).reshape(B, G, n_cmp, CMP_LEN * NSA_DH)
        return jax.nn.gelu(blk @ w1) @ w2

    kc = rmsnorm(compress(heads(k_cmp, G), cmp_pe[0], cmp_w1[0], cmp_w2[0]), k_gain[0])
    vc = compress(heads(v_cmp, G), cmp_pe[1], cmp_w1[1], cmp_w2[1])
    cmp_end = jnp.arange(n_cmp) * CMP_STRIDE + CMP_LEN - 1
    n_slc = T // SLC_LEN
    top_n = min(SLC_TOPK, n_slc)
    k_sb = rmsnorm(heads(k_slc, G), k_gain[1]).reshape(B, G, n_slc, SLC_LEN, NSA_DH)
    v_sb = heads(v_slc, G).reshape(B, G, n_slc, SLC_LEN, NSA_DH)
    pad_w = ((0, 0), (0, 0), (WIN, 0), (0, 0))
    k_wp = jnp.pad(rmsnorm(heads(k_win, G), k_gain[2]), pad_w)
    v_wp = jnp.pad(heads(v_win, G), pad_w)
    gates = jax.nn.sigmoid(gate_logit.astype(f32)).reshape(B, T, NSA_HEADS, 3).transpose(0, 2, 1, 3)
    gates = gates.reshape(B, G, R, T, 3)
    slopes = alibi_slopes(NSA_HEADS).reshape(1, G, R, 1, 1)
    gather_blocks = jax.vmap(jax.vmap(lambda blocks, ix: blocks[ix]))

    def query_block(b):
        t0 = b * Q_BLOCK
        qb = lax.dynamic_slice_in_dim(qg, t0, Q_BLOCK, axis=3)
        gb = lax.dynamic_slice_in_dim(gates, t0, Q_BLOCK, axis=3)
        t = t0 + jnp.arange(Q_BLOCK)
        dist_c = t[:, None] - cmp_end[None, :]
        s_c = jnp.einsum('bgrtd,bgnd->bgrtn', qb, kc) - slopes * dist_c
        p_c = masked_softmax(s_c, dist_c >= 0)
        o_c = jnp.einsum('bgrtn,bgnd->bgrtd', p_c, vc)
        imp = cmp_to_slc(jnp.sum(p_c, axis=2), n_slc)
        blk_id = jnp.arange(n_slc)[None, :]
        cur = (t // SLC_LEN)[:, None]
        forced = (blk_id == 0) | (blk_id == cur) | (blk_id == cur - 1)
        imp = jnp.where(forced, jnp.inf, jnp.where(blk_id <= cur, imp, -jnp.inf))
        _, sel = lax.top_k(imp, top_n)
        ks = gather_blocks(k_sb, sel).reshape(B, G, Q_BLOCK, top_n * SLC_LEN, NSA_DH)
        vs = gather_blocks(v_sb, sel).reshape(B, G, Q_BLOCK, top_n * SLC_LEN, NSA_DH)
        pos = (sel[..., None] * SLC_LEN + jnp.arange(SLC_LEN)).reshape(B, G, Q_BLOCK, top_n * SLC_LEN)
        dist_s = (t[:, None] - pos)[:, :, None]
        s_s = jnp.einsum('bgrtd,bgtmd->bgrtm', qb, ks) - slopes * dist_s
        p_s = masked_softmax(s_s, dist_s >= 0)
        o_s = jnp.einsum('bgrtm,bgtmd->bgrtd', p_s, vs)
        kw = lax.dynamic_slice_in_dim(k_wp, t0, WIN + Q_BLOCK, axis=2)
        vw = lax.dynamic_slice_in_dim(v_wp, t0, WIN + Q_BLOCK, axis=2)
        pos_w = t0 - WIN + jnp.arange(WIN + Q_BLOCK)
        dist_w = t[:, None] - pos_w[None, :]
        valid_w = (dist_w >= 0) & (dist_w < WIN) & (pos_w[None, :] >= 0)
        s_w = jnp.einsum('bgrtd,bgsd->bgrts', qb, kw) - slopes * dist_w
        p_w = masked_softmax(s_w, valid_w)
        o_w = jnp.einsum('bgrts,bgsd->bgrtd', p_w, vw)
        return gb[..., 0:1] * o_c + gb[..., 1:2] * o_s + gb[..., 2:3] * o_w

    o = lax.map(query_block, jnp.arange(T // Q_BLOCK))
    o = o.transpose(1, 0, 4, 2, 3, 5).reshape(B, T, NSA_HEADS, NSA_DH)
    return rmsnorm(o, out_gain.reshape(NSA_HEADS, NSA_DH)).reshape(B, T, NSA_WIDTH)


def peer_ffn(h, wq, sub_keys, u, v):
    B, T, D = h.shape
    f32 = jnp.float32
    tokens = h.reshape(-1, PEER_TOKEN_BLOCK, D)

    def token_block(xt):
        q = (xt @ wq).astype(f32).reshape(PEER_TOKEN_BLOCK, PEER_HEADS, 2, PEER_DKEY // 2)
        s = jnp.einsum('thcd,hckd->thck', q, sub_keys.astype(f32))
        top_s, top_i = lax.top_k(s, PEER_TOPK)
        cand = top_s[:, :, 0, :, None] + top_s[:, :, 1, None, :]
        best_s, best_c = lax.top_k(cand.reshape(PEER_TOKEN_BLOCK, PEER_HEADS, PEER_TOPK * PEER_TOPK), PEER_TOPK)
        i1 = jnp.take_along_axis(top_i[:, :, 0], best_c // PEER_TOPK, axis=-1)
        i2 = jnp.take_along_axis(top_i[:, :, 1], best_c % PEER_TOPK, axis=-1)
        expert = i1 * PEER_NKEYS + i2
        w = jax.nn.softmax(best_s, axis=-1)
        act = jax.nn.gelu(jnp.einsum('thkd,td->thk', u[expert].astype(f32), xt.astype(f32)))
        return jnp.einsum('thk,thkd->td', w * act, v[expert].astype(f32))

    return lax.map(token_block, tokens).reshape(B, T, D).astype(h.dtype)


def setup_inputs(seed: int = 0) -> dict:
    key = jax.random.key(seed)
    ks = jax.random.split(key, 21)
    n = jax.random.normal
    f32 = jnp.float32

    def gain(k, shape):
        return 1.0 + 0.02 * n(k, shape, f32)

    return {
        'x': n(ks[0], (BATCH, SEQ, D_MODEL), f32),
        'p': n(ks[1], (DEPTH, BATCH, SEQ, PLE_DIM), f32),
        'mix_norm': gain(ks[2], (DEPTH, D_MODEL)),
        'w_in': n(ks[3], (DEPTH, D_MODEL, D_IN), f32) * D_MODEL ** -0.5,
        'hg_lb_logits': 0.1 * n(ks[4], (DEPTH + 1, HG_WIDTH), f32),
        'hg_out_norm': gain(ks[5], (DEPTH, HG_WIDTH)),
        'nsa_q_norm': gain(ks[6], (DEPTH, NSA_DH)),
        'nsa_k_norm': gain(ks[7], (DEPTH, 3, NSA_DH)),
        'cmp_pe': 0.02 * n(ks[8], (DEPTH, 2, CMP_LEN, NSA_DH), f32),
        'cmp_w1': n(ks[9], (DEPTH, 2, CMP_LEN * NSA_DH, NSA_DH), f32) * (CMP_LEN * NSA_DH) ** -0.5,
        'cmp_w2': n(ks[10], (DEPTH, 2, NSA_DH, NSA_DH), f32) * NSA_DH ** -0.5,
        'nsa_out_norm': gain(ks[11], (DEPTH, NSA_WIDTH)),
        'w_out': n(ks[12], (DEPTH, D_MIX, D_MODEL), f32) * D_MIX ** -0.5,
        'ffn_norm': gain(ks[13], (DEPTH, D_MODEL)),
        'peer_wq': n(ks[14], (DEPTH, D_MODEL, PEER_HEADS * PEER_DKEY), f32) * D_MODEL ** -0.5,
        'peer_keys': n(ks[15], (DEPTH, PEER_HEADS, 2, PEER_NKEYS, PEER_DKEY // 2), f32) * (PEER_DKEY // 2) ** -0.5,
        'peer_u': n(ks[16], (DEPTH, PEER_N, D_MODEL), f32) * D_MODEL ** -0.5,
        'peer_v': n(ks[17], (DEPTH, PEER_N, D_MODEL), f32) * PEER_HEADS ** -0.5,
        'ple_proj': n(ks[18], (DEPTH, PLE_DIM, D_MODEL), f32) * PLE_DIM ** -0.5,
        'ple_gate_norm': gain(ks[19], (DEPTH, D_MODEL)),
        'ple_gate_w': n(ks[20], (DEPTH, D_MODEL, D_MODEL), f32) * D_MODEL ** -0.5,
    }


def reference(x, p, mix_norm, w_in, hg_lb_logits, hg_out_norm, nsa_q_norm, nsa_k_norm,
              cmp_pe, cmp_w1, cmp_w2, nsa_out_norm, w_out, ffn_norm, peer_wq, peer_keys,
              peer_u, peer_v, ple_proj, ple_gate_norm, ple_gate_w):
    lb_table = jnp.cumsum(jax.nn.softmax(hg_lb_logits.astype(jnp.float32), axis=0), axis=0)
    h = x
    for i in range(DEPTH):
        a = rmsnorm(h, mix_norm[i])
        proj = a @ w_in[i]
        (hg_q, hg_f, hg_i, hg_g, n_q, n_kc, n_vc, n_ks, n_vs, n_kw, n_vw, n_gate) = jnp.split(proj, SPLIT_POINTS, axis=-1)
        o_hg = hgrn2_mixer(hg_q, hg_f, hg_i, hg_g, lb_table[i], hg_out_norm[i])
        o_nsa = nsa_mixer(n_q, n_kc, n_vc, n_ks, n_vs, n_kw, n_vw, n_gate,
                          nsa_q_norm[i], nsa_k_norm[i], cmp_pe[i], cmp_w1[i], cmp_w2[i], nsa_out_norm[i])
        mixed = jnp.concatenate([o_hg, o_nsa], axis=-1).astype(h.dtype)
        h = h + mixed @ w_out[i]
        h = h + peer_ffn(rmsnorm(h, ffn_norm[i]), peer_wq[i], peer_keys[i], peer_u[i], peer_v[i])
        gate = jax.nn.sigmoid(rmsnorm(h, ple_gate_norm[i]) @ ple_gate_w[i])
        h = h + (p[i] @ ple_proj[i]) * gate
    return h
```

```python
import numpy as np
import concourse.bass as bass
import concourse.mybir as mybir
from concourse.bass_utils import run_bass_kernel_spmd

F32 = mybir.dt.float32
BF16 = mybir.dt.bfloat16
U32 = mybir.dt.uint32
ALU = mybir.AluOpType
AF = mybir.ActivationFunctionType
AX = mybir.AxisListType

ENGS = ("sp", "act", "dve", "pool", "pe")
EPS = 1e-6
T = 8192
NEG = -30000.0
PSUM_KEYS = {"pj0", "pj1", "trp", "sc0", "sc1", "oacc", "uacc", "hgp", "pa", "pb", "pc", "pd", "pe_", "pf", "pg", "ph"}


class Prog:
    def __init__(self, nc):
        self.nc = nc
        self.ops = []
        self.last_w = {}
        self.readers = {}
        self.eng_count = {e: 0 for e in ENGS}
        self.dma_count = {}
        self.last_dma = {}

    def add(self, eng, fn, r=(), w=(), dma=None):
        idx = len(self.ops)
        w = list(w) + [k for k in r if k in PSUM_KEYS]
        r = [k for k in r if k not in PSUM_KEYS]
        deps = set()
        for k in r:
            if k in self.last_w:
                deps.add(self.last_w[k])
        for k in w:
            if k in self.last_w:
                deps.add(self.last_w[k])
            for x in self.readers.get(k, ()):
                deps.add(x)
        if dma is not None:
            if dma in self.last_dma:
                deps.add(self.last_dma[dma])
            self.last_dma[dma] = idx
        op = dict(eng=eng, fn=fn, deps=deps, dma=dma)
        op["dmawait"] = {}
        for di in deps:
            dk = self.ops[di]["dma"]
            if dk is not None:
                op["dmawait"][dk] = self.dma_count[dk]
        if dma is None:
            self.eng_count[eng] += 1
            op["val"] = self.eng_count[eng]
            op["pos"] = self.eng_count[eng]
        else:
            self.dma_count[dma] = self.dma_count.get(dma, 0) + 16
            op["val"] = self.dma_count[dma]
            op["pos"] = self.eng_count[eng]
        self.ops.append(op)
        for k in r:
            self.readers.setdefault(k, []).append(idx)
        for k in w:
            self.last_w[k] = idx
            self.readers[k] = []
        return idx

    def emit(self, final_keys=()):
        nc = self.nc
        ops = self.ops
        fin = set()
        for k in final_keys:
            if k in self.last_w:
                fin.add(self.last_w[k])
        per_eng = {e: [] for e in ENGS}
        for i, op in enumerate(ops):
            per_eng[op["eng"]].append(i)

        def needs_wait(op, d):
            if d["dma"] is None and d["eng"] == op["eng"] and op["dma"] is None:
                if op["eng"] == "pe":
                    return False
            return True

        marked = set(fin)
        for i, op in enumerate(ops):
            if op["dma"] is not None:
                marked.add(i)
        for op in ops:
            for di in op["deps"]:
                if needs_wait(op, ops[di]):
                    marked.add(di)
        cnt = {}
        for i, op in enumerate(ops):
            if i not in marked:
                op["sval"] = None
                continue
            key = ("dma", op["dma"]) if op["dma"] is not None else ("eng", op["eng"])
            cnt[key] = cnt.get(key, 0) + (16 if op["dma"] is not None else 1)
            op["sval"] = cnt[key]
            op["skey"] = key
        sems = {}
        for key in cnt:
            sems[key] = nc.alloc_semaphore("sem_%s_%s" % key)
        scratch = {}
        for e in ("act", "dve", "pool"):
            scratch[e] = nc.alloc_sbuf_tensor("sigscr_" + e, [128, 2], F32)

        def run(engname, eng):
            known = {}
            for i in per_eng[engname]:
                op = ops[i]
                need = {}
                for di in op["deps"]:
                    d = ops[di]
                    if not needs_wait(op, d):
                        continue
                    v0 = d["sval"]
                    if d["dma"] is not None:
                        v0 = op["dmawait"][d["dma"]]
                    need[d["skey"]] = max(need.get(d["skey"], 0), v0)
                for sk, v in need.items():
                    if known.get(sk, 0) >= v:
                        continue
                    eng.wait_ge(sems[sk], v)
                    known[sk] = v
                ins = op["fn"](eng)
                if op["sval"] is not None:
                    if op["dma"] is not None:
                        ins.then_inc(sems[op["skey"]], 16)
                    else:
                        ins.then_inc(sems[op["skey"]], 1)
            if engname == "sp":
                need = {}
                for di in fin:
                    d = ops[di]
                    v0 = cnt[d["skey"]] if d["dma"] is not None else d["sval"]
                    need[d["skey"]] = max(need.get(d["skey"], 0), v0)
                for sk, v in need.items():
                    eng.wait_ge(sems[sk], v)

        with nc.Block() as block:
            @block.sync
            def _(e):
                run("sp", e)

            @block.scalar
            def _(e):
                run("act", e)

            @block.vector
            def _(e):
                run("dve", e)

            @block.gpsimd
            def _(e):
                run("pool", e)

            @block.tensor
            def _(e):
                run("pe", e)


class Ctx:
    def __init__(self):
        self.nc = bass.Bass("TRN2", target_bir_lowering=False)
        self.P = Prog(self.nc)
        self.outs = []

    def din(self, name, shape, dt=F32):
        return self.nc.dram_tensor(name, list(shape), dt, kind="ExternalInput").ap()

    def dout(self, name, shape, dt=F32):
        self.outs.append(name)
        return self.nc.dram_tensor(name, list(shape), dt, kind="ExternalOutput").ap()

    def sb(self, name, shape, dt=F32):
        return self.nc.alloc_sbuf_tensor("s_" + name, list(shape), dt)

    def ps(self, name, shape, dt=F32):
        return self.nc.alloc_psum_tensor("p_" + name, list(shape), dt)


def rms_tm(C, src, dst, npart, G, D, gain_b, tag, r, w):
    P = C.P
    key = "rms_" + tag
    if not hasattr(C, "_rms"):
        C._rms = {}
    if tag not in C._rms:
        C._rms[tag] = (C.sb("rmsq_" + tag, [128, G * D]), C.sb("rmss_" + tag, [128, G]), C.sb("rmsl_" + tag, [128, G]), C.sb("rmsr_" + tag, [128, G]))
    if not hasattr(C, "epsT"):
        C.epsT = C.sb("epsT", [128, 1])
        P.add("pool", lambda e: e.memset(C.epsT[:], EPS), w=["consts"])
    sq, ss, sl, sr = C._rms[tag]
    sqv = sq[0:npart, :].rearrange("p (g d) -> p g d", d=D)
    ssv = ss[0:npart, :]
    slv = sl[0:npart, :]
    srv = sr[0:npart, :]
    epsv = C.epsT[0:npart, 0:1]
    P.add("dve", lambda e: e.tensor_tensor(sqv, src, src, ALU.mult), r=r, w=[key + "q"])
    P.add("dve", lambda e: e.tensor_reduce(ssv, sqv, AX.X, ALU.add), r=[key + "q"], w=[key + "s"])
    P.add("act", lambda e: e.activation(slv, ssv, AF.Ln, bias=epsv, scale=1.0 / D), r=[key + "s", "consts"], w=[key + "l"])
    P.add("act", lambda e: e.activation(srv, slv, AF.Exp, scale=-0.5), r=[key + "l"], w=[key + "r"])
    for gi in range(G):
        P.add("dve", lambda e, gi=gi: e.scalar_tensor_tensor(dst[:, gi, :], src[:, gi, :], srv[:, gi:gi + 1], gain_b[:, gi, :], ALU.mult, ALU.mult),
              r=list(r) + [key + "r", "consts"], w=w)


def make_ident(C):
    P = C.P
    iot = C.sb("iot", [128, 128])
    iop = C.sb("iop", [128, 1])
    idf = C.sb("identf", [128, 128])
    ident = C.sb("ident", [128, 128], BF16)
    P.add("pool", lambda e: e.iota(iot[:], [[1, 128]], base=0, channel_multiplier=0,
                                   allow_small_or_imprecise_dtypes=True), w=["iot"])
    P.add("pool", lambda e: e.iota(iop[:], [[0, 1]], base=0, channel_multiplier=1,
                                   allow_small_or_imprecise_dtypes=True), w=["iop"])
    P.add("dve", lambda e: e.tensor_scalar(idf[:], iot[:], iop[:, 0:1], None, ALU.is_equal), r=["iot", "iop"], w=["identf"])
    P.add("dve", lambda e: e.tensor_copy(ident[:], idf[:]), r=["identf"], w=["ident"])
    C.iot, C.iop, C.identf, C.ident = iot, iop, idf, ident


NW = 1164
DEBUG = False


def build_l1(n_tiles=16):
    C = Ctx()
    nc, P = C.nc, C.P
    x = C.din("x", [T, 1024])
    w = C.din("w", [1024, NW])
    gmix_d = C.din("gmix", [1, 1024])
    lbl_d = C.din("lbl", [128, 2])
    hgon_d = C.din("hgon", [1, 128])
    g6_d = C.din("g6", [1, 384])
    gkc_d = C.din("gkc", [1, 64])
    pe_d = C.din("pe", [64, 64])
    w1_d = C.din("w1", [64, 2 * 32 * 64])
    w2_d = C.din("w2", [64, 128])
    nson_d = C.din("nson", [1, 128])
    qaug_d = C.din("qaug", [4, 4 * T])
    kaug_d = C.din("kaug", [4, T])
    kcaug_d = C.din("kcaug", [4, 512])
    wc_d = C.din("wc", [128, 512])
    addm_d = C.din("addm", [T, 128])
    tri_d = C.din("tri", [64, 64])
    o_hg = C.dout("o_hg", [T, 128])
    o_ns = C.dout("o_ns", [T, 128])

    make_ident(C)
    ident = C.ident
    OWN_R = {0: 0, 1: 1}

    xs = [C.sb("xs0", [128, 1024]), C.sb("xs1", [128, 1024])]
    wb = C.sb("wb", [128, 8 * NW], BF16)
    wbv = wb[:].rearrange("p (c n) -> p c n", n=NW)
    wst = C.sb("wst", [128, NW])
    for dc in range(8):
        P.add("sp", lambda e, dc=dc: e.dma_start(out=wst[:], in_=w[dc * 128:(dc + 1) * 128, :]), w=["wst"], dma="wst")
        P.add("dve", lambda e, dc=dc: e.tensor_copy(wbv[:, dc, :], wst[:]), r=["wst"], w=["wb"])
    gmix = C.sb("gmix", [128, 1024])
    P.add("sp", lambda e: e.dma_start(out=gmix[:], in_=gmix_d.partition_broadcast(128)), w=["consts"], dma="c0")
    hgon = C.sb("hgon", [128, 128])
    P.add("sp", lambda e: e.dma_start(out=hgon[:], in_=hgon_d.partition_broadcast(128)), w=["consts"], dma="c0")
    g6 = C.sb("g6", [128, 384])
    P.add("sp", lambda e: e.dma_start(out=g6[:], in_=g6_d.partition_broadcast(128)), w=["consts"], dma="c0")
    gkc = C.sb("gkc", [128, 64])
    P.add("sp", lambda e: e.dma_start(out=gkc[:], in_=gkc_d.partition_broadcast(128)), w=["consts"], dma="c0")
    nson = C.sb("nson", [128, 128])
    P.add("sp", lambda e: e.dma_start(out=nson[:], in_=nson_d.partition_broadcast(128)), w=["consts"], dma="c0")
    nson4 = C.sb("nson4", [128, 512])
    for j4 in range(4):
        P.add("dve", lambda e, j4=j4: e.tensor_copy(nson4[:, j4 * 128:(j4 + 1) * 128], nson[:]), r=["consts"], w=["consts"])
    P.add("dve", lambda e: e.tensor_scalar(g6[:, 0:256], g6[:, 0:256], 0.125, None, ALU.mult), r=["consts"], w=["consts"])
    lbl = C.sb("lbl", [128, 2])
    P.add("sp", lambda e: e.dma_start(out=lbl[:], in_=lbl_d), w=["lbl"], dma="c0")
    lb = C.sb("lb", [128, 2])
    P.add("dve", lambda e: e.tensor_tensor(lb[:, 0:1], lbl[:, 0:1], lbl[:, 1:2], ALU.subtract), r=["lbl"], w=["lb"])
    P.add("act", lambda e: e.activation(lb[:, 0:1], lb[:, 0:1], AF.Sigmoid), r=["lb"], w=["lb"])
    P.add("dve", lambda e: e.tensor_scalar(lb[:, 1:2], lb[:, 0:1], -1.0, 1.0, ALU.mult, ALU.add), r=["lb"], w=["lb"])
    tri = C.sb("tri", [64, 64])
    P.add("sp", lambda e: e.dma_start(out=tri[:], in_=tri_d), w=["consts"], dma="c0")
    w1b = C.sb("w1b", [64, 4096], BF16)
    w1v = w1b[:].rearrange("p (k j o) -> p k j o", k=2, j=32)
    for q4 in range(4):
        P.add("sp", lambda e, q4=q4: e.dma_start(out=xs[0][0:64, :], in_=w1_d[:, q4 * 1024:(q4 + 1) * 1024]), w=["xs0"], dma="x0")
        P.add("dve", lambda e, q4=q4: e.tensor_copy(w1b[:, q4 * 1024:(q4 + 1) * 1024], xs[0][0:64, :]), r=["xs0"], w=["cw"])
    st2 = C.sb("st2", [64, 192])
    w2b = C.sb("w2b", [64, 128], BF16)
    peb = C.sb("peb", [64, 128], BF16)
    P.add("sp", lambda e: e.dma_start(out=st2[:, 0:128], in_=w2_d), w=["st2"], dma="c1")
    P.add("sp", lambda e: e.dma_start(out=st2[:, 128:192], in_=pe_d), w=["st2"], dma="c1")
    P.add("dve", lambda e: e.tensor_copy(w2b[:], st2[:, 0:128]), r=["st2"], w=["cw"])
    pebv2 = peb[:].rearrange("p (q two) -> p q two", two=2)
    P.add("dve", lambda e: e.tensor_copy(pebv2[:, :, 0], st2[:, 128:192]), r=["st2"], w=["cw"])
    P.add("dve", lambda e: e.tensor_copy(pebv2[:, :, 1], st2[:, 128:192]), r=["st2"], w=["cw"])
    wcs = C.sb("wcs", [128, 512])
    wcb = C.sb("wcb", [128, 512], BF16)
    P.add("sp", lambda e: e.dma_start(out=wcs[:], in_=wc_d), w=["wcs"], dma="c1")
    P.add("dve", lambda e: e.tensor_copy(wcb[:], wcs[:]), r=["wcs"], w=["cw"])
    wcv = wcb[:].rearrange("p (c s) -> p c s", s=128)
    vca = C.sb("vca", [128, 4 * 65], BF16)
    vcav = vca[:].rearrange("p (c d) -> p c d", d=65)
    vsa = C.sb("vsa", [128, 64 * 65], BF16)
    vsav = vsa[:].rearrange("p (k d) -> p k d", d=65)
    vwa = C.sb("vwa", [128, 8 * 65], BF16)
    vwav = vwa[:].rearrange("p (k d) -> p k d", d=65)
    for (t_, v_, k_) in ((vca, vcav, "vca"), (vsa, vsav, "vsa"), (vwa, vwav, "vwa")):
        P.add("pool", lambda e, t_=t_: e.memset(t_[:], 0.0), w=[k_])
        P.add("pool", lambda e, v_=v_: e.memset(v_[:, :, 64:65], 1.0), w=[k_])
    ksT = C.sb("ksT", [68, T], BF16)
    kwT = C.sb("kwT", [68, 1024], BF16)
    kcT = C.sb("kcT", [68, 512], BF16)
    P.add("pool", lambda e: e.memset(ksT[0:64, :], 0.0), w=["ksT"])
    P.add("pool", lambda e: e.memset(kwT[:], 0.0), w=["kwT"])
    P.add("pool", lambda e: e.memset(kcT[0:64, :], 0.0), w=["kcT"])
    for q8 in range(8):
        P.add("sp", lambda e, q8=q8: e.dma_start(out=xs[1][64:68, :], in_=kaug_d[:, q8 * 1024:(q8 + 1) * 1024]), w=["xs1"], dma="x1")
        P.add("dve", lambda e, q8=q8: e.tensor_copy(ksT[64:68, q8 * 1024:(q8 + 1) * 1024], xs[1][64:68, :]), r=["xs1"], w=["ksT"])
    P.add("sp", lambda e: e.dma_start(out=xs[1][64:68, 0:512], in_=kcaug_d), w=["xs1"], dma="x1")
    P.add("dve", lambda e: e.tensor_copy(kcT[64:68, :], xs[1][64:68, 0:512]), r=["xs1"], w=["kcT"])
    crT = C.sb("crT", [64, 2 * 528], BF16)
    crv = crT[:].rearrange("p (k t) -> p k t", k=2)
    P.add("pool", lambda e: e.memset(crT[:], 0.0), w=["crT"])
    Eb = C.sb("Eb", [128, 64 * 128], BF16)
    Ebv = Eb[:].rearrange("p (k m) -> p k m", m=128)
    P.add("pool", lambda e: e.memset(Eb[:], 1.0), w=["Eb"])
    P.add("pool", lambda e: e.affine_select(Ebv, Ebv, [[128, 64], [1, 128]], ALU.is_ge, 0.0, base=0, channel_multiplier=-64), r=["Eb"], w=["Eb"])
    P.add("pool", lambda e: e.affine_select(Ebv, Ebv, [[-128, 64], [-1, 128]], ALU.is_ge, 0.0, base=63, channel_multiplier=64), r=["Eb"], w=["Eb"])
    mk = C.sb("mk", [128, 18 * 128], BF16)
    MASKS = {}
    P.add("pool", lambda e: e.memset(mk[:], 1e30), w=["mk"])
    mspecs = [(0, -1, 1), (-1, 1, -1)] + [(128 * k - 15, -16, 1) for k in range(16)]
    for mi, (b0, cm, st) in enumerate(mspecs):
        mv = mk[:, mi * 128:(mi + 1) * 128]
        MASKS[(b0, cm, st)] = mv
        P.add("pool", lambda e, mv=mv, b0=b0, cm=cm, st=st: e.affine_select(mv, mv, [[st, 128]], ALU.is_ge, 0.0, base=b0, channel_multiplier=cm),
              r=["mk"], w=["mk"])
    mkdone = C.sb("mkdone", [128, 8])
    P.add("pool", lambda e: e.memset(mkdone[:], 0.0), r=["mk", "Eb"], w=["masks"])
    pj = [C.ps("pj0", [128, 512]), C.ps("pj1", [128, 512])]
    trp = C.ps("trp", [128, 1024], BF16)
    sc = [C.ps("sc0", [128, 512]), C.ps("sc1", [128, 512])]
    oacc = C.ps("oacc", [128, 512])
    uacc = C.ps("uacc", [128, 512])
    hgp = C.ps("hgp", [128, 512])
    cbias = C.sb("cbias", [64, 4])
    pev = peb[:].rearrange("p (k j two) -> p k j two", k=2, two=2)
    for kv in range(2):
        for j in range(32):
            P.add("pe", lambda e, kv=kv, j=j: e.matmul(pj[0][0:64, 2 * kv:2 * kv + 2], w1v[:, kv, j, :], pev[:, kv, j, :],
                                                      start=(j == 0 and kv == 0), stop=(j == 31), skip_group_check=True), r=["cw"], w=["pj0"])
    P.add("dve", lambda e: e.tensor_copy(cbias[:], pj[0][0:64, 0:4]), r=["pj0"], w=["cbias"])

    Sf = C.sb("Sf", [128, 128])
    Sb = C.sb("Sb", [128, 128], BF16)
    P.add("dve", lambda e: e.memset(Sf[:], 0.0), w=["Sf"])
    P.add("dve", lambda e: e.memset(Sb[:], 0.0), w=["Sb"])
    cmask = C.sb("cmask", [128, 512])
    P.add("dve", lambda e: e.memset(cmask[:], 1.0), w=["cmask"])
    P.add("dve", lambda e: e.memset(cmask[:].rearrange("p (c s) -> p c s", s=64)[:, :, 0:1], 0.0), w=["cmask"])

    xn = C.sb("xn", [128, 1024], BF16)
    xss = C.sb("xss", [128, 1])
    xsl2 = C.sb("xsl2", [128, 1])
    xsr = C.sb("xsr", [128, 1])
    epsx = C.sb("epsx", [128, 1])
    P.add("pool", lambda e: e.memset(epsx[:], EPS), w=["consts"])
    xnT = C.sb("xnT", [128, 8 * 512], BF16)
    xnTv = xnT[:].rearrange("p (c t) -> p c t", t=512)
    qT = C.sb("qT", [68, 4 * 512], BF16)
    qTv = qT[:].rearrange("p (h t) -> p h t", t=512)
    qst = C.sb("qst", [68, 512])
    tmf = C.sb("tmf", [128, 652])
    qkn = C.sb("qkn", [128, 384], BF16)
    craw = C.sb("craw", [128, 128], BF16)
    gates = C.sb("gates", [128, 48])
    gatv = gates[:].rearrange("p (j c) -> p j c", c=12)
    hq = C.sb("hq", [128, 512])
    hk = C.sb("hk", [128, 512])
    hG = C.sb("hG", [128, 512])
    hE = C.sb("hE", [128, 512])
    hD = C.sb("hD", [128, 512])
    hE3 = C.sb("hE3", [128, 512])
    QtT = C.sb("QtT", [128, 512], BF16)
    KtT = C.sb("KtT", [128, 512], BF16)
    QhT = C.sb("QhT", [128, 512], BF16)
    KhT = C.sb("KhT", [128, 512], BF16)
    vch = C.sb("vch", [64, 8 * 128], BF16)
    vchv = vch[:].rearrange("p (c d) -> p c d", d=128)
    gch = C.sb("gch", [64, 8 * 128])
    gchv = gch[:].rearrange("p (c d) -> p c d", d=128)
    och = C.sb("och", [64, 8 * 128])
    ochv = och[:].rearrange("p (c d) -> p c d", d=128)
    oout = och
    ooutv = ochv
    ATm = C.sb("ATm", [64, 64], BF16)
    Khc = C.sb("Khc", [64, 128], BF16)
    hx = C.sb("hx", [64, 32])
    hu = C.sb("hu", [64, 32])
    gHT = C.sb("gHT", [64, 32], BF16)
    gHV = C.sb("gHV", [64, 128], BF16)
    P.add("dve", lambda e: e.memset(gHV[:], 0.0), w=["gHV"])
    kcf = C.sb("kcf", [32, 64])
    kcn = C.sb("kcn", [32, 64], BF16)
    vcn = C.sb("vcn", [32, 64], BF16)
    pT = [C.sb("pT0", [128, 512], BF16), C.sb("pT1", [128, 512], BF16)]
    ob = C.sb("ob", [128, 4 * 65])
    obv = ob[:].rearrange("p (j d) -> p j d", d=65)
    rz = C.sb("rz", [128, 4])
    coef = C.sb("coef", [128, 4])
    imp = C.sb("imp", [128, 512])
    impv = imp[:].rearrange("p (j s) -> p j s", s=128)
    utmp = C.sb("utmp", [128, 512])
    utv = utmp[:].rearrange("p (j s) -> p j s", s=128)
    addm = C.sb("addm", [128, 512])
    addv = addm[:].rearrange("p (j s) -> p j s", s=128)
    top8 = C.sb("top8", [128, 16])
    impw = C.sb("impw", [128, 128])
    negs = C.sb("negs", [128, 512], BF16)
    negv = negs[:].rearrange("p (j s) -> p j s", s=128)
    negT = C.sb("negT", [128, 512], BF16)
    acc = C.sb("acc", [128, 512])
    accv = acc[:].rearrange("p (j r d) -> p j r d", j=4, r=2)
    atmp = C.sb("atmp", [128, 256])
    atv = atmp[:].rearrange("p (j d) -> p j d", d=64)
    nout = acc

    x_t = x.rearrange("(n p) f -> n p f", p=128)
    o_ns_t = o_ns.rearrange("(n p) f -> p n f", p=128)
    o_hg_t = o_hg.rearrange("(n p) f -> p n f", p=64)
    addm_t = addm_d.rearrange("(n p) s -> p n s", p=128)
    qaug_v = qaug_d.rearrange("r (h t) -> r h t", t=T)

    pj_rot = [0]
    dbg_cnt = [0]
    if DEBUG:
        dbg_ob = C.dout("dbg_ob", [8, 128, 260])

    def next_pj():
        pj_rot[0] ^= 1
        return pj[pj_rot[0]], "pj%d" % pj_rot[0]

    sc_rot = [0]

    def attn_branch(a, h, kTbuf, kTkey, vview, vkey, kts, conds, first_flag, extra_mask=False, imp_c=False, kmap=lambda k: k):
        qh = qTv[:, h, :]
        for kt in kts:
            modes = []
            for j in range(4):
                mode = "full"
                part = []
                for (bf, cm, st) in conds:
                    b0 = bf(kt, j)
                    lo = b0 + min(0, cm * 127) + min(0, st * 127)
                    hi = b0 + max(0, cm * 127) + max(0, st * 127)
                    if hi < 0:
                        mode = "skip"
                        break
                    if lo < 0:
                        part.append((b0, cm, st))
                if mode != "skip" and part:
                    mode = part
                modes.append(mode)
            val = [j for j in range(4) if modes[j] != "skip"]
            if not val:
                continue
            jlo, jhi = min(val), max(val)
            sc_rot[0] ^= 1
            s_ps, s_key = sc[sc_rot[0]], "sc%d" % sc_rot[0]
            p_sb, p_key = pT[sc_rot[0]], "pT%d" % sc_rot[0]
            cols = slice(jlo * 128, (jhi + 1) * 128)
            P.add("pe", lambda e, kt=kt, s_ps=s_ps, cols=cols: e.matmul(
                s_ps[:, cols], kTbuf[:, kmap(kt) * 128:(kmap(kt) + 1) * 128], qh[:, cols], start=True, stop=not extra_mask),
                r=[kTkey, "qT"], w=[s_key])
            if extra_mask:
                P.add("pe", lambda e, kt=kt, s_ps=s_ps, cols=cols: e.matmul(
                    s_ps[:, cols], Ebv[:, kt, :], negT[:, cols], start=False, stop=True),
                    r=["masks", "negT"], w=[s_key])
            P.add("act", lambda e, s_ps=s_ps, p_sb=p_sb, cols=cols: e.activation(p_sb[:, cols], s_ps[:, cols], AF.Exp),
                  r=[s_key], w=[p_key])
            for j in val:
                if modes[j] != "full":
                    for (b0, cm, st) in modes[j]:
                        mv = MASKS[(b0, cm, st)]
                        P.add("dve", lambda e, p_sb=p_sb, j=j, mv=mv: e.tensor_tensor(
                            p_sb[:, j * 128:(j + 1) * 128], p_sb[:, j * 128:(j + 1) * 128], mv, ALU.min),
                            r=[p_key, "masks"], w=[p_key])
            for j in val:
                st_flag = first_flag[0]
                first_flag[0] = False
                P.add("pe", lambda e, p_sb=p_sb, j=j, kt=kt, st_flag=st_flag: e.matmul(
                    oacc[:, j * 65:(j + 1) * 65], p_sb[:, j * 128:(j + 1) * 128], vview[:, kmap(kt), :],
                    start=st_flag, stop=True, skip_group_check=True), r=[p_key, vkey], w=["oacc"])
                if imp_c:
                    st2 = first_flag[1]
                    first_flag[1] = False
                    P.add("pe", lambda e, p_sb=p_sb, j=j, kt=kt, st2=st2: e.matmul(
                        uacc[:, j * 128:(j + 1) * 128], p_sb[:, j * 128:(j + 1) * 128], wcv[:, kt, :],
                        start=st2, stop=True, skip_group_check=True), r=[p_key, "cw"], w=["uacc"])

    def finish_branch(a, r_loc, gate_col, first, do_imp=False):
        P.add("act", lambda e: e.activation(ob[:], oacc[:, 0:260], AF.Copy), r=["oacc"], w=["ob"])
        if DEBUG and a == n_tiles - 1:
            di = dbg_cnt[0]
            dbg_cnt[0] += 1
            P.add("sp", lambda e, di=di: e.dma_start(out=dbg_ob[di], in_=ob[:]), r=["ob"], w=["dbg_ob"], dma="dbg2")
        P.add("dve", lambda e: e.tensor_scalar(rz[:], obv[:, :, 64], 1e-30, None, ALU.max), r=["ob"], w=["rz"])
        P.add("dve", lambda e: e.reciprocal(rz[:], rz[:]), r=["rz"], w=["rz"])
        if do_imp:
            P.add("dve", lambda e: e.tensor_tensor(utv, uacc[:].rearrange("p (j s) -> p j s", s=128),
                                                   rz[:].unsqueeze(2).to_broadcast([128, 4, 128]), ALU.mult),
                  r=["uacc", "rz"], w=["utmp"])
            P.add("dve", lambda e: e.tensor_tensor(imp[:], imp[:], utmp[:], ALU.add), r=["utmp", "imp"], w=["imp"])
        if r_loc is None:
            return
        P.add("dve", lambda e: e.tensor_tensor(coef[:], rz[:], gatv[:, :, gate_col], ALU.mult), r=["rz", "gates"], w=["coef"])
        if first:
            P.add("dve", lambda e: e.tensor_tensor(accv[:, :, r_loc, :], obv[:, :, 0:64],
                                                   coef[:].unsqueeze(2).to_broadcast([128, 4, 64]), ALU.mult),
                  r=["ob", "coef"], w=["acc"])
        else:
            P.add("dve", lambda e: e.tensor_tensor(atv, obv[:, :, 0:64],
                                                   coef[:].unsqueeze(2).to_broadcast([128, 4, 64]), ALU.mult),
                  r=["ob", "coef"], w=["atmp"])
            P.add("dve", lambda e: e.tensor_tensor(accv[:, :, r_loc, :], accv[:, :, r_loc, :], atv, ALU.add),
                  r=["atmp", "acc"], w=["acc"])

    def hgrn_chunk(a, c):
        cs = slice(c * 64, (c + 1) * 64)
        P.add("pe", lambda e: e.matmul(hgp[0:64, 0:64], KtT[:, cs], QtT[:, cs], start=True, stop=True),
              r=["KtT", "QtT"], w=["hgp"])
        P.add("dve", lambda e: e.tensor_tensor(ATm[:], hgp[0:64, 0:64], tri[:], ALU.mult), r=["hgp", "consts"], w=["ATm"])
        P.add("pe", lambda e: e.transpose(trp[0:64, 0:128], KhT[:, cs], ident[:]), r=["KhT", "ident"], w=["trp"])
        P.add("act", lambda e: e.activation(Khc[:], trp[0:64, 0:128], AF.Copy), r=["trp"], w=["Khc"])
        P.add("pe", lambda e: e.matmul(hgp[0:64, 64:192], ATm[:], vchv[:, c, :], start=True, stop=False),
              r=["ATm", "vch"], w=["hgp"])
        P.add("pe", lambda e: e.matmul(hgp[0:64, 64:192], QhT[:, cs], Sb[:], start=False, stop=True),
              r=["QhT", "Sb"], w=["hgp"])
        P.add("act", lambda e: e.activation(ochv[:, c, :], hgp[0:64, 64:192], AF.Copy), r=["hgp"], w=["och"])
        P.add("pe", lambda e: e.matmul(hgp[:, 192:320], Khc[:], vchv[:, c, :], start=True, stop=True),
              r=["Khc", "vch"], w=["hgp"])
        P.add("dve", lambda e: e.scalar_tensor_tensor(Sf[:], Sf[:], hE3[:, c * 64 + 63:c * 64 + 64], hgp[:, 192:320],
                                                      ALU.mult, ALU.add), r=["Sf", "hE3", "hgp"], w=["Sf"])
        P.add("dve", lambda e: e.tensor_copy(Sb[:], Sf[:]), r=["Sf"], w=["Sb"])

    for a in range(n_tiles):
        for j in range(4):
            slot = (a * 4 + j) % 2
            xsl = xs[slot]
            P.add("sp", lambda e, xsl=xsl, a=a, j=j: e.dma_start(out=xsl[:], in_=x_t[a * 4 + j]), w=["xs%d" % slot], dma="x%d" % slot)
            P.add("act", lambda e, xsl=xsl: e.activation(xn[:], xsl[:], AF.Square, accum_out=xss[:, 0:1]), r=["xs%d" % slot], w=["xss", "xn"])
            P.add("act", lambda e: e.activation(xsl2[:], xss[:], AF.Ln, bias=epsx[:, 0:1], scale=1.0 / 1024), r=["xss", "consts"], w=["xsl2"])
            P.add("act", lambda e: e.activation(xsr[:], xsl2[:], AF.Exp, scale=-0.5), r=["xsl2"], w=["xsr"])
            P.add("dve", lambda e, xsl=xsl: e.scalar_tensor_tensor(xn[:], xsl[:], xsr[:, 0:1], gmix[:], ALU.mult, ALU.mult),
                  r=["xs%d" % slot, "xsr", "consts"], w=["xn"])
            for dc in range(8):
                P.add("pe", lambda e, dc=dc: e.transpose(trp[:, dc * 128:(dc + 1) * 128], xn[:, dc * 128:(dc + 1) * 128], ident[:]),
                      r=["xn", "ident"], w=["trp"])
            P.add("act", lambda e, j=j: e.activation(xnTv[:, :, j * 128:(j + 1) * 128],
                                                    trp[:].rearrange("p (c t) -> p c t", t=128), AF.Copy),
                  r=["trp"], w=["xnT"])
        pq, pqk = next_pj()
        for dc in range(8):
            P.add("pe", lambda e, dc=dc, pq=pq: e.matmul(pq[:, :], wbv[:, dc, 0:128], xnTv[:, dc, :], start=(dc == 0), stop=(dc == 7)),
                  r=["wb", "xnT"], w=[pqk])
        P.add("act", lambda e, pq=pq: e.activation(hq[:], pq[:, :], AF.Silu), r=[pqk], w=["hq"])
        P.add("dve", lambda e: e.tensor_scalar(hq[:], hq[:], 128 ** -0.5, None, ALU.mult), r=["hq"], w=["hq"])
        pf, pfk = next_pj()
        for dc in range(8):
            P.add("pe", lambda e, dc=dc, pf=pf: e.matmul(pf[:, :], wbv[:, dc, 128:256], xnTv[:, dc, :], start=(dc == 0), stop=(dc == 7)),
                  r=["wb", "xnT"], w=[pfk])
        P.add("act", lambda e, pf=pf: e.activation(hk[:], pf[:, :], AF.Sigmoid), r=[pfk], w=["hk"])
        P.add("dve", lambda e: e.tensor_scalar(hk[:], hk[:], lb[:, 1:2], lb[:, 0:1], ALU.mult, ALU.add), r=["hk", "lb"], w=["hk"])
        P.add("act", lambda e: e.activation(hD[:], hk[:], AF.Ln), r=["hk"], w=["hD"])
        P.add("dve", lambda e: e.tensor_scalar(hk[:], hk[:], -1.0, 1.0, ALU.mult, ALU.add), r=["hk"], w=["hk"])
        P.add("dve", lambda e: e.tensor_tensor_scan(hG[:], cmask[:], hD[:], 0.0, ALU.mult, ALU.add), r=["cmask", "hD"], w=["hG"])
        hGv = hG[:].rearrange("p (c s) -> p c s", s=64)
        hDv = hD[:].rearrange("p (c s) -> p c s", s=64)
        P.add("dve", lambda e: e.tensor_tensor(hDv, hGv, hGv[:, :, 31:32].to_broadcast([128, 8, 64]), ALU.subtract), r=["hG"], w=["hD"])
        P.add("act", lambda e: e.activation(hE[:], hD[:], AF.Exp), r=["hD"], w=["hE"])
        P.add("dve", lambda e: e.tensor_tensor(QtT[:], hq[:], hE[:], ALU.mult), r=["hq", "hE"], w=["QtT"])
        P.add("act", lambda e: e.activation(hE[:], hD[:], AF.Exp, scale=-1.0), r=["hD", "QtT"], w=["hE"])
        P.add("dve", lambda e: e.tensor_tensor(KtT[:], hk[:], hE[:], ALU.mult), r=["hk", "hE"], w=["KtT"])
        P.add("act", lambda e: e.activation(hE3[:], hG[:], AF.Exp), r=["hG"], w=["hE3"])
        P.add("dve", lambda e: e.tensor_tensor(QhT[:], hq[:], hE3[:], ALU.mult), r=["hq", "hE3"], w=["QhT"])
        P.add("dve", lambda e: e.tensor_tensor(hDv, hGv[:, :, 63:64].to_broadcast([128, 8, 64]), hGv, ALU.subtract), r=["hG", "KtT"], w=["hD"])
        P.add("act", lambda e: e.activation(hE[:], hD[:], AF.Exp), r=["hD", "KtT"], w=["hE"])
        P.add("dve", lambda e: e.tensor_tensor(KhT[:], hk[:], hE[:], ALU.mult), r=["hk", "hE"], w=["KhT"])
        for c2 in range(4):
            pv, pvk = next_pj()
            for cc in range(2):
                c = c2 * 2 + cc
                for dc in range(8):
                    P.add("pe", lambda e, dc=dc, c=c, cc=cc, pv=pv: e.matmul(
                        pv[0:64, cc * 256:(cc + 1) * 256], xnTv[:, dc, c * 64:(c + 1) * 64], wbv[:, dc, 256:512],
                        start=(dc == 0 and cc == 0), stop=(dc == 7), skip_group_check=True), r=["wb", "xnT"], w=[pvk])
            pvv = pv[0:64, :].rearrange("p (c d) -> p c d", d=256)
            P.add("dve", lambda e, pvv=pvv, c2=c2: e.tensor_copy(vchv[:, c2 * 2:c2 * 2 + 2, :], pvv[:, :, 0:128]), r=[pvk], w=["vch"])
            P.add("act", lambda e, pvv=pvv, c2=c2: e.activation(gchv[:, c2 * 2:c2 * 2 + 2, :], pvv[:, :, 128:256], AF.Silu), r=[pvk], w=["gch"])
        for h4 in range(4):
            P.add("sp", lambda e, a=a, h4=h4: e.dma_start(out=qst[64:68, :], in_=qaug_v[:, h4, a * 512:(a + 1) * 512]), w=["qst"], dma="qa")
            P.add("dve", lambda e, h4=h4: e.tensor_copy(qTv[64:68, h4, :], qst[64:68, :]), r=["qst"], w=["qT"])
        for j in range(4):
            kt = a * 4 + j
            p0, p0k = next_pj()
            for dc in range(8):
                P.add("pe", lambda e, dc=dc, j=j, p0=p0: e.matmul(p0[:, :], xnTv[:, dc, j * 128:(j + 1) * 128], wbv[:, dc, 512:1024],
                                                                 start=(dc == 0), stop=(dc == 7)), r=["wb", "xnT"], w=[p0k])
            P.add("act", lambda e, p0=p0: e.activation(tmf[:, 0:512], p0[:, :], AF.Copy), r=[p0k], w=["tmf"])
            p1, p1k = next_pj()
            for dc in range(8):
                P.add("pe", lambda e, dc=dc, j=j, p1=p1: e.matmul(p1[:, 0:140], xnTv[:, dc, j * 128:(j + 1) * 128], wbv[:, dc, 1024:1164],
                                                                 start=(dc == 0), stop=(dc == 7)), r=["wb", "xnT"], w=[p1k])
            P.add("dve", lambda e, p1=p1, kt=kt: e.tensor_copy(vsav[:, kt, 0:64], p1[:, 0:64]), r=[p1k], w=["vsa"])
            P.add("dve", lambda e, p1=p1, kt=kt: e.tensor_copy(vwav[:, kt % 8, 0:64], p1[:, 64:128]), r=[p1k], w=["vwa"])
            P.add("act", lambda e, p1=p1, j=j: e.activation(gatv[:, j, :], p1[:, 128:140], AF.Sigmoid), r=[p1k], w=["gates"])
            rms_tm(C, tmf[:, 0:384].rearrange("p (g d) -> p g d", d=64), qkn[:].rearrange("p (g d) -> p g d", d=64),
                   128, 6, 64, g6[:].rearrange("p (g d) -> p g d", d=64), "qk", r=["tmf"], w=["qkn"])
            P.add("dve", lambda e: e.tensor_copy(craw[:], tmf[:, 384:512]), r=["tmf"], w=["craw"])
            for gidx in range(6):
                P.add("pe", lambda e, gidx=gidx: e.transpose(trp[0:64, gidx * 128:(gidx + 1) * 128], qkn[:, gidx * 64:(gidx + 1) * 64], ident[:]),
                      r=["qkn", "ident"], w=["trp"])
            for gidx in range(2):
                P.add("pe", lambda e, gidx=gidx: e.transpose(trp[0:64, (6 + gidx) * 128:(7 + gidx) * 128], craw[:, gidx * 64:(gidx + 1) * 64], ident[:]),
                      r=["craw", "ident"], w=["trp"])
            trv = trp[0:64, :].rearrange("p (g t) -> p g t", t=128)
            P.add("act", lambda e, j=j, trv=trv: e.activation(qTv[0:64, :, j * 128:(j + 1) * 128], trv[:, 0:4, :], AF.Copy), r=["trp"], w=["qT"])
            P.add("dve", lambda e, kt=kt, trv=trv: e.tensor_copy(ksT[0:64, kt * 128:(kt + 1) * 128], trv[:, 4, :]), r=["trp"], w=["ksT"])
            P.add("dve", lambda e, kt=kt, trv=trv: e.tensor_copy(kwT[0:64, (kt % 8) * 128:(kt % 8 + 1) * 128], trv[:, 5, :]), r=["trp"], w=["kwT"])
            P.add("dve", lambda e, kt=kt: e.tensor_copy(kwT[64:68, (kt % 8) * 128:(kt % 8 + 1) * 128], ksT[64:68, kt * 128:(kt + 1) * 128]), r=["ksT"], w=["kwT"])
            P.add("act", lambda e, j=j, trv=trv: e.activation(crv[:, :, 16 + j * 128:16 + (j + 1) * 128], trv[:, 6:8, :], AF.Copy), r=["trp"], w=["crT"])
        for kv in range(2):
            ph, phk = next_pj()
            src = crv[:, kv, :]
            for jj in range(32):
                rhs = src[:, jj: jj + 497: 16]
                P.add("pe", lambda e, jj=jj, kv=kv, rhs=rhs, ph=ph: e.matmul(ph[0:64, 0:32], w1v[:, kv, jj, :], rhs, start=(jj == 0), stop=(jj == 31)),
                      r=["cw", "crT"], w=[phk])
            P.add("act", lambda e, kv=kv, ph=ph: e.activation(hx[:], ph[0:64, 0:32], AF.Identity, bias=cbias[:, 2 * kv:2 * kv + 1]), r=[phk, "cbias"], w=["hx"])
            P.add("dve", lambda e: e.tensor_tensor(hu[:], hx[:], hx[:], ALU.mult), r=["hx"], w=["hu"])
            P.add("dve", lambda e: e.tensor_scalar(hu[:], hu[:], 0.044715, 1.0, ALU.mult, ALU.add), r=["hu"], w=["hu"])
            P.add("dve", lambda e: e.tensor_tensor(hu[:], hu[:], hx[:], ALU.mult), r=["hu", "hx"], w=["hu"])
            P.add("act", lambda e: e.activation(hu[:], hu[:], AF.Tanh, scale=0.7978845608028654), r=["hu"], w=["hu"])
            P.add("dve", lambda e: e.tensor_scalar(hu[:], hu[:], 0.5, 0.5, ALU.mult, ALU.add), r=["hu"], w=["hu"])
            po, pok = next_pj()
            if kv == 0:
                P.add("dve", lambda e: e.tensor_tensor(gHT[:], hu[:], hx[:], ALU.mult), r=["hu", "hx"], w=["gHT"])
                P.add("pe", lambda e, po=po: e.matmul(po[0:32, 0:64], gHT[:], w2b[:, 0:64], start=True, stop=True),
                      r=["gHT", "cw"], w=[pok])
            else:
                k4 = a % 4
                P.add("dve", lambda e, k4=k4: e.tensor_tensor(gHV[:, k4 * 32:(k4 + 1) * 32], hu[:], hx[:], ALU.mult), r=["hu", "hx"], w=["gHV"])
                P.add("pe", lambda e, po=po: e.matmul(po[:, 0:64], gHV[:], w2b[:, 64:128], start=True, stop=True),
                      r=["gHV", "cw"], w=[pok])
            if kv == 0:
                P.add("act", lambda e, po=po: e.activation(kcf[:], po[0:32, 0:64], AF.Copy), r=[pok], w=["kcf"])
                rms_tm(C, kcf[:].rearrange("p (g d) -> p g d", d=64), kcn[:].rearrange("p (g d) -> p g d", d=64), 32, 1, 64,
                       gkc[0:32, :].rearrange("p (g d) -> p g d", d=64), "kc", r=["kcf"], w=["kcn"])
                P.add("pe", lambda e: e.transpose(trp[0:64, 0:32], kcn[:], ident[0:32, 0:32]), r=["kcn", "ident"], w=["trp"])
                P.add("dve", lambda e, a=a: e.tensor_copy(kcT[0:64, a * 32:(a + 1) * 32], trp[0:64, 0:32]), r=["trp"], w=["kcT"])
            else:
                P.add("act", lambda e, po=po, a=a: e.activation(vcav[32 * (a % 4):32 * (a % 4) + 32, a // 4, 0:64],
                                                                po[32 * (a % 4):32 * (a % 4) + 32, 0:64], AF.Copy), r=[pok], w=["vca"])
        P.add("act", lambda e: e.activation(crv[:, :, 0:16], crv[:, :, 512:528], AF.Copy), r=["crT"], w=["crT"])
        items = []
        c_max = (512 * a + 511 - 15) // 2048
        P.add("dve", lambda e: e.memset(imp[:], 0.0), w=["imp"])
        P.add("sp", lambda e, a=a: e.dma_start(out=addv, in_=addm_t[:, a * 4:(a + 1) * 4, :]), w=["addm"], dma="am")

        def cmp_head(h, a=a, c_max=c_max):
            ff = [True, True]
            attn_branch(a, h, kcT, "kcT", vcav, "vca", list(range(c_max + 1)),
                        [(lambda kt, j, a=a: 512 * a + 128 * j - 2048 * kt - 15, -16, 1)], ff, imp_c=True)
            r_loc = OWN_R.get(h)
            finish_branch(a, r_loc, 3 * h + 0, True, do_imp=True)

        for h in range(4):
            items.append(lambda h=h: cmp_head(h))

        def topk_stage(a=a):
            P.add("dve", lambda e: e.tensor_tensor(imp[:], imp[:], addm[:], ALU.add), r=["imp", "addm"], w=["imp"])
            for j in range(4):
                P.add("dve", lambda e, j=j: e.max(top8[:, 0:8], impv[:, j, :]), r=["imp"], w=["top8"])
                P.add("dve", lambda e, j=j: e.match_replace(impw[:], top8[:, 0:8], impv[:, j, :], -3e30), r=["imp", "top8"], w=["impw"])
                P.add("dve", lambda e: e.max(top8[:, 8:16], impw[:]), r=["impw"], w=["top8"])
                P.add("dve", lambda e, j=j: e.tensor_scalar(negv[:, j, :], impv[:, j, :], top8[:, 15:16], NEG, ALU.is_lt, ALU.mult),
                      r=["imp", "top8"], w=["negs"])
                P.add("pe", lambda e, j=j: e.transpose(trp[:, j * 128:(j + 1) * 128], negv[:, j, :], ident[:]), r=["negs", "ident"], w=["trp"])
            P.add("act", lambda e: e.activation(negT[:], trp[:, 0:512], AF.Copy), r=["trp"], w=["negT"])

        items.append(topk_stage)

        def slc_head(h, a=a):
            ff = [True, True]
            attn_branch(a, h, ksT, "ksT", vsav, "vsa", list(range(4 * a + 4)),
                        [(lambda kt, j, a=a: 512 * a + 128 * j - 128 * kt, -1, 1)], ff, extra_mask=True)
            finish_branch(a, OWN_R[h], 3 * h + 1, False)

        def win_head(h, a=a):
            ff = [True, True]
            attn_branch(a, h, kwT, "kwT", vwav, "vwa", [k for k in range(4 * a - 4, 4 * a + 4) if k >= 0],
                        [(lambda kt, j, a=a: 512 * a + 128 * j - 128 * kt, -1, 1),
                         (lambda kt, j, a=a: 128 * kt - 512 * a - 128 * j + 511, 1, -1)], ff, kmap=lambda k: k % 8)
            finish_branch(a, OWN_R[h], 3 * h + 2, False)

        for h in OWN_R:
            items.append(lambda h=h: slc_head(h))
            items.append(lambda h=h: win_head(h))
        n_it = len(items)
        ci = 0
        for ii, it in enumerate(items):
            it()
            tgt = (ii + 1) * 8 // n_it
            while ci < tgt:
                hgrn_chunk(a, ci)
                ci += 1
        rms_tm(C, acc[:].rearrange("p (g d) -> p g d", d=64), nout[:].rearrange("p (g d) -> p g d", d=64), 128, 8, 64,
               nson4[:].rearrange("p (g d) -> p g d", d=64), "no", r=["acc"], w=["acc"])
        P.add("sp", lambda e, a=a: e.dma_start(out=o_ns_t[:, a * 4:(a + 1) * 4, :], in_=nout[:].rearrange("p (j f) -> p j f", f=128)),
              r=["acc"], w=["o_ns"], dma="on")
        rms_tm(C, ochv, ooutv, 64, 8, 128, hgon[0:64, :].unsqueeze(1).to_broadcast([64, 8, 128]), "ho", r=["och"], w=["och"])
        P.add("dve", lambda e: e.tensor_tensor(oout[:], oout[:], gch[:], ALU.mult), r=["och", "gch"], w=["och"])
        P.add("sp", lambda e, a=a: e.dma_start(out=o_hg_t[:, a * 8:(a + 1) * 8, :], in_=ooutv), r=["och"], w=["o_hg"], dma="oh")

    if DEBUG:
        dbg_s = C.dout("dbg_s", [128, 24])
        ds_ = C.sb("ds_", [128, 24])
        sq_, ss_, sl_, sr_ = C._rms["no"]
        P.add("dve", lambda e: e.tensor_copy(ds_[:, 0:8], ss_[:]), r=["rms_nos"], w=["ds_"])
        P.add("dve", lambda e: e.tensor_copy(ds_[:, 8:16], sl_[:]), r=["rms_nol"], w=["ds_"])
        P.add("dve", lambda e: e.tensor_copy(ds_[:, 16:24], sr_[:]), r=["rms_nor"], w=["ds_"])
        P.add("sp", lambda e: e.dma_start(out=dbg_s, in_=ds_[:]), r=["ds_"], w=["dbg_s"], dma="dbg")
        dbg_g = C.dout("dbg_g", [128, 48 + 512])
        dg = C.sb("dg", [128, 48 + 512])
        P.add("dve", lambda e: e.tensor_copy(dg[:, 0:48], gates[:]), r=["gates"], w=["dg"])
        P.add("dve", lambda e: e.tensor_copy(dg[:, 48:560], negs[:]), r=["negs"], w=["dg"])
        P.add("sp", lambda e: e.dma_start(out=dbg_g, in_=dg[:]), r=["dg"], w=["dbg_g"], dma="dbg")
        P.emit(final_keys=["o_ns", "o_hg", "dbg_g", "dbg_ob", "dbg_s"])
    else:
        P.emit(final_keys=["o_ns", "o_hg"])
    assert nc.sbuf_bytes_remaining >= 20 * 1024, nc.sbuf_bytes_remaining
    return C


def l1_in_maps(inp):
    x = np.asarray(inp["x"], np.float32)
    w_in = np.asarray(inp["w_in"], np.float32)[0]
    slopes = 2.0 ** (-8.0 * np.arange(1, 9) / 8)
    tt = np.arange(T)
    kaug = np.stack([tt // 64, tt % 64, np.ones(T), np.ones(T)]).astype(np.float32)
    npos = 16 * np.arange(512) + 15
    kcaug = np.stack([npos // 64, npos % 64, np.ones(512), np.ones(512)]).astype(np.float32)
    kcaug[0, 0] = -65536.0
    kcaug[1, 0] = 0.0
    wts = [1.0, 2.0, 2.0, 2.0, 1.0]
    wc = np.zeros((512, 128), np.float32)
    for npr in range(1, 512):
        n = npr - 1
        for o in range(5):
            if (n - o) % 4 == 0 and 0 <= (n - o) // 4 < 128:
                wc[npr, (n - o) // 4] = wts[o]
    wc = wc.reshape(4, 128, 128).transpose(1, 0, 2).reshape(128, 512).copy()
    cur = (tt // 64)[:, None]
    sidx = np.arange(128)[None, :]
    addm = np.where(sidx > cur, -1e30, 0.0)
    addm = np.where((sidx == 0) | (sidx == cur) | (sidx == cur - 1), 1e30, addm).astype(np.float32)
    tri = np.triu(np.ones((64, 64), np.float32))
    pe = np.asarray(inp["cmp_pe"], np.float32)[0].transpose(2, 0, 1).reshape(64, 64).copy()
    w1 = np.asarray(inp["cmp_w1"], np.float32)[0].reshape(2, 32, 64, 64).transpose(2, 0, 1, 3).reshape(64, 4096).copy()
    w2 = np.asarray(inp["cmp_w2"], np.float32)[0].transpose(1, 0, 2).reshape(64, 128).copy()
    qn = np.asarray(inp["nsa_q_norm"], np.float32)[0]
    kn = np.asarray(inp["nsa_k_norm"], np.float32)[0]
    g6 = np.concatenate([qn, qn, qn, qn, kn[1], kn[2]])[None, :].copy()
    gkc = kn[0][None, :].copy()
    maps = []
    meta = []
    for c in range(8):
        b, u = c // 4, c % 4
        g, hp = u // 2, u % 2
        order = [2 * hp, 2 * hp + 1, 2 * (1 - hp), 2 * (1 - hp) + 1]
        cols = []
        for base in (0, 512, 1024, 1536):
            cols.append(base + u * 128 + np.arange(128))
        for r in order:
            cols.append(2048 + (4 * g + r) * 64 + np.arange(64))
        for base in (2816, 3072, 2560, 2688, 2944, 3200):
            cols.append(base + g * 64 + np.arange(64))
        for r in order:
            cols.append(3328 + (4 * g + r) * 3 + np.arange(3))
        cols = np.concatenate(cols)
        assert cols.shape[0] == NW
        qaug = np.zeros((4, 4, T), np.float32)
        for hs, r in enumerate(order):
            s = slopes[4 * g + r]
            qaug[0, hs] = s * 64
            qaug[1, hs] = s
            qaug[2, hs] = -s * 64 * (tt // 64)
            qaug[3, hs] = -s * (tt % 64)
        h0 = 4 * g + 2 * hp
        maps.append({
            "x": np.ascontiguousarray(x[b]),
            "w": np.ascontiguousarray(w_in[:, cols]),
            "gmix": np.asarray(inp["mix_norm"], np.float32)[0][None, :].copy(),
            "lbl": np.ascontiguousarray(np.asarray(inp["hg_lb_logits"], np.float32)[:, u * 128:(u + 1) * 128].T),
            "hgon": np.asarray(inp["hg_out_norm"], np.float32)[0, u * 128:(u + 1) * 128][None, :].copy(),
            "g6": g6, "gkc": gkc, "pe": pe, "w1": w1, "w2": w2,
            "nson": np.asarray(inp["nsa_out_norm"], np.float32)[0, h0 * 64:h0 * 64 + 128][None, :].copy(),
            "qaug": qaug.reshape(4, 4 * T), "kaug": kaug, "kcaug": kcaug, "wc": wc, "addm": addm, "tri": tri,
        })
        meta.append((b, u, h0))
    return maps, meta


def build_l2(n_tiles=16, n_tok=128):
    C = Ctx()
    nc, P = C.nc, C.P
    NT = 2048
    x = C.din("x", [NT, 1024])
    mixed = C.din("mixed", [NT, 1024])
    pin = C.din("p", [NT, 256])
    w_out = C.din("w_out", [1024, 1024])
    wq = C.din("wq", [1024, 2048])
    gw = C.din("gw", [1024, 1024])
    ple = C.din("ple", [256, 1024])
    keysT = C.din("keysT", [128, 2048])
    fn_d = C.din("fn", [1, 1024])
    gn_d = C.din("gn", [1, 1024])
    pu = C.din("pu", [16384, 1024])
    pv = C.din("pv", [16384, 1024])
    out = C.dout("out", [NT, 1024])
    make_ident(C)
    ident, identf = C.ident, C.identf

    pa = C.ps("pa", [128, 512])
    pb = C.ps("pb", [128, 512])
    trp = C.ps("trp", [128, 1024], BF16)
    bcp = C.ps("pc", [128, 1024])
    pop = C.ps("pd", [128, 1024])
    PSUM_KEYS.update({"pa", "pb", "trp", "pc", "pd"})
    prot = [0]

    def nps():
        prot[0] ^= 1
        return (pa, "pa") if prot[0] else (pb, "pb")

    stg = C.sb("stg", [128, 1024])
    wob = C.sb("wob", [128, 8 * 1024], BF16)
    wov = wob[:].rearrange("p (c n) -> p c n", n=1024)
    wqb = C.sb("wqb", [128, 8 * 2048], BF16)
    wqv = wqb[:].rearrange("p (c n) -> p c n", n=2048)
    gwb = C.sb("gwb", [128, 8 * 1024], BF16)
    gwv = gwb[:].rearrange("p (c n) -> p c n", n=1024)
    pleb = C.sb("pleb", [128, 2 * 1024], BF16)
    plev = pleb[:].rearrange("p (c n) -> p c n", n=1024)
    kTb = C.sb("kTb", [128, 2048], BF16)
    kTv = kTb[:].rearrange("p (h k) -> p h k", k=128)

    def load_w(src, rows, dstv, ncol, tag):
        for rc in range(rows // 128):
            for nh in range(ncol // 1024):
                P.add("sp", lambda e, rc=rc, nh=nh: e.dma_start(out=stg[:], in_=src[rc * 128:(rc + 1) * 128, nh * 1024:(nh + 1) * 1024]),
                      w=["stg"], dma="stg")
                P.add("dve", lambda e, rc=rc, nh=nh: e.tensor_copy(dstv[:, rc, nh * 1024:(nh + 1) * 1024], stg[:]), r=["stg"], w=[tag])

    load_w(w_out, 1024, wov, 1024, "wob")
    load_w(wq, 1024, wqv, 2048, "wqb")
    load_w(gw, 1024, gwv, 1024, "gwb")
    load_w(ple, 256, plev, 1024, "pleb")
    for nh in range(2):
        P.add("sp", lambda e, nh=nh: e.dma_start(out=stg[:], in_=keysT[:, nh * 1024:(nh + 1) * 1024]), w=["stg"], dma="stg")
        P.add("dve", lambda e, nh=nh: e.tensor_copy(kTb[:, nh * 1024:(nh + 1) * 1024], stg[:]), r=["stg"], w=["kTb"])
    fnb = C.sb("fnb", [128, 1024])
    gnb = C.sb("gnb", [128, 1024])
    P.add("sp", lambda e: e.dma_start(out=fnb[:], in_=fn_d.partition_broadcast(128)), w=["consts"], dma="cA")
    P.add("sp", lambda e: e.dma_start(out=gnb[:], in_=gn_d.partition_broadcast(128)), w=["consts"], dma="cA")
    iot16 = C.sb("iot16", [128, 16])
    P.add("pool", lambda e: e.iota(iot16[:], [[1, 16]], base=0, channel_multiplier=0, allow_small_or_imprecise_dtypes=True), w=["consts"])
    thr16 = C.sb("thr16", [128, 16])
    P.add("dve", lambda e: e.tensor_scalar(thr16[:], iot16[:], 1.0, 16.0, ALU.add, ALU.mult), r=["consts"], w=["consts"])
    Zc = C.sb("Zc", [128, 256], BF16)
    P.add("pool", lambda e: e.memset(Zc[:], 0.0), w=["consts"])
    P.add("pool", lambda e: e.memset(Zc[:, 127:128], 1.0), w=["consts"])

    xt = C.sb("xt", [128, 1024])
    mt = C.sb("mt", [128, 1024])
    mb = C.sb("mb", [128, 1024], BF16)
    mT = C.sb("mT", [128, 1024], BF16)
    mTv = mT[:].rearrange("p (c t) -> p c t", t=128)
    h1 = C.sb("h1", [128, 1024])
    hn = C.sb("hn", [128, 1024], BF16)
    hnT = C.sb("hnT", [128, 1024], BF16)
    hnTv = hnT[:].rearrange("p (c t) -> p c t", t=128)
    qT = C.sb("qT", [128, 2048], BF16)
    qTv = qT[:].rearrange("p (h t) -> p h t", t=128)
    S = C.sb("S", [128, 512])
    Sv = S[:].rearrange("p (h k) -> p h k", k=128)
    swk = C.sb("swk", [128, 256])
    tv = C.sb("tv", [128, 256])
    tvv = tv[:].rearrange("p (h c k) -> p h c k", h=8, c=2)
    ti = C.sb("ti", [128, 256], U32)
    tif = C.sb("tif", [128, 256])
    tifv = tif[:].rearrange("p (h c k) -> p h c k", h=8, c=2)
    cand = C.sb("cand", [128, 2048])
    candv = cand[:].rearrange("p (h a b) -> p h a b", h=8, a=16)
    candh = cand[:].rearrange("p (h q) -> p h q", h=8)
    bs = C.sb("bs", [128, 128])
    bsv = bs[:].rearrange("p (h k) -> p h k", k=16)
    bci = C.sb("bci", [128, 128], U32)
    bcf = C.sb("bcf", [128, 128])
    av = C.sb("av", [128, 128])
    bv = C.sb("bv", [128, 128])
    i1 = C.sb("i1", [128, 128])
    i2 = C.sb("i2", [128, 128])
    exf = C.sb("exf", [128, 128])
    eidx = C.sb("eidx", [128, 128], U32)
    wgt = C.sb("wgt", [128, 128])
    ssum = C.sb("ssum", [128, 8])
    wT = C.sb("wT", [128, 128])
    actT = C.sb("actT", [128, 128])
    gx = C.sb("gx", [128, 128])
    CT = C.sb("CT", [128, 128])
    ug = [C.sb("ug%d" % i, [128, 1024]) for i in range(3)]
    vg = [C.sb("vg%d" % i, [128, 1024]) for i in range(2)]
    vgb = [C.sb("vgb%d" % i, [128, 1024], BF16) for i in range(2)]
    selb = [C.sb("selb%d" % i, [128, 128], BF16) for i in range(2)]
    Cm = [C.sb("Cm%d" % i, [128, 128], BF16) for i in range(2)]
    junk = C.sb("junk", [128, 1024], BF16)
    pt = C.sb("pt", [128, 256])
    ptb = C.sb("ptb", [128, 256], BF16)
    pT = C.sb("pT", [128, 256], BF16)
    pTv = pT[:].rearrange("p (c t) -> p c t", t=128)
    gate = C.sb("gate", [128, 1024])

    x_t = x.rearrange("(n p) f -> n p f", p=128)
    m_t = mixed.rearrange("(n p) f -> n p f", p=128)
    p_t = pin.rearrange("(n p) f -> n p f", p=128)
    o_t = out.rearrange("(n p) f -> n p f", p=128)

    def transpose8(src, dstv, skey, dkey, nch=8):
        for dc in range(nch):
            P.add("pe", lambda e, dc=dc: e.transpose(trp[:, dc * 128:(dc + 1) * 128], src[:, dc * 128:(dc + 1) * 128], ident[:]),
                  r=[skey, "ident"], w=["trp"])
        P.add("act", lambda e: e.activation(dstv, trp[:, 0:nch * 128].rearrange("p (c t) -> p c t", t=128), AF.Copy), r=["trp"], w=[dkey])

    def gelu_tanh(xa, tmp, outa, keyx, keyt, keyo):
        P.add("dve", lambda e: e.tensor_tensor(tmp, xa, xa, ALU.mult), r=[keyx], w=[keyt])
        P.add("dve", lambda e: e.tensor_scalar(tmp, tmp, 0.044715, 1.0, ALU.mult, ALU.add), r=[keyt], w=[keyt])
        P.add("dve", lambda e: e.tensor_tensor(tmp, tmp, xa, ALU.mult), r=[keyt, keyx], w=[keyt])
        P.add("act", lambda e: e.activation(tmp, tmp, AF.Tanh, scale=0.7978845608028654), r=[keyt], w=[keyt])
        P.add("dve", lambda e: e.tensor_scalar(tmp, tmp, 0.5, 0.5, ALU.mult, ALU.add), r=[keyt], w=[keyt])
        P.add("dve", lambda e: e.tensor_tensor(outa, tmp, xa, ALU.mult), r=[keyt, keyx], w=[keyo])

    for it in range(n_tiles):
        P.add("sp", lambda e, it=it: e.dma_start(out=xt[:], in_=x_t[it]), w=["xt"], dma="xt")
        P.add("sp", lambda e, it=it: e.dma_start(out=mt[:], in_=m_t[it]), w=["mt"], dma="mt")
        P.add("sp", lambda e, it=it: e.dma_start(out=pt[:], in_=p_t[it]), w=["pt"], dma="pt")
        P.add("act", lambda e: e.activation(mb[:], mt[:], AF.Copy), r=["mt"], w=["mb"])
        transpose8(mb, mTv, "mb", "mT")
        for half in range(2):
            ps_, pk = nps()
            for dc in range(8):
                P.add("pe", lambda e, dc=dc, half=half, ps_=ps_: e.matmul(ps_[:, :], mTv[:, dc, :], wov[:, dc, half * 512:(half + 1) * 512],
                                                                       start=(dc == 0), stop=(dc == 7)), r=["mT", "wob"], w=[pk])
            P.add("dve", lambda e, half=half, ps_=ps_: e.tensor_tensor(h1[:, half * 512:(half + 1) * 512], ps_[:, :], xt[:, half * 512:(half + 1) * 512], ALU.add),
                  r=[pk, "xt"], w=["h1"])
        rms_tm(C, h1[:].rearrange("p (g d) -> p g d", g=1), hn[:].rearrange("p (g d) -> p g d", g=1), 128, 1, 1024,
               fnb[:].rearrange("p (g d) -> p g d", g=1), "f", r=["h1"], w=["hn"])
        transpose8(hn, hnTv, "hn", "hnT")
        for q4 in range(4):
            ps_, pk = nps()
            for hh in range(4):
                hc = q4 * 4 + hh
                for dc in range(8):
                    P.add("pe", lambda e, dc=dc, hc=hc, hh=hh, ps_=ps_: e.matmul(ps_[:, hh * 128:(hh + 1) * 128], wqv[:, dc, hc * 128:(hc + 1) * 128], hnTv[:, dc, :],
                                                                             start=(dc == 0), stop=(dc == 7), skip_group_check=True), r=["wqb", "hnT"], w=[pk])
            P.add("act", lambda e, q4=q4, ps_=ps_: e.activation(qT[:, q4 * 512:(q4 + 1) * 512], ps_[:, :], AF.Copy), r=[pk], w=["qT"])
        for q4 in range(4):
            ps_, pk = nps()
            for hh in range(4):
                hc = q4 * 4 + hh
                P.add("pe", lambda e, hc=hc, hh=hh, ps_=ps_: e.matmul(ps_[:, hh * 128:(hh + 1) * 128], qTv[:, hc, :], kTv[:, hc, :], start=True, stop=True,
                                                                  skip_group_check=True), r=["qT", "kTb"], w=[pk])
            P.add("act", lambda e, ps_=ps_: e.activation(S[:], ps_[:, :], AF.Copy), r=[pk], w=["S"])
            for hh in range(4):
                hc = q4 * 4 + hh
                P.add("dve", lambda e, hc=hc, hh=hh: e.max(tv[:, hc * 16:hc * 16 + 8], Sv[:, hh, :]), r=["S"], w=["tv"])
                P.add("dve", lambda e, hc=hc, hh=hh: e.match_replace(swk[:, 0:128], tv[:, hc * 16:hc * 16 + 8], Sv[:, hh, :], -3e30), r=["S", "tv"], w=["swk"])
                P.add("dve", lambda e, hc=hc: e.max(tv[:, hc * 16 + 8:hc * 16 + 16], swk[:, 0:128]), r=["swk"], w=["tv"])
                P.add("dve", lambda e, hc=hc, hh=hh: e.max_index(ti[:, hc * 16:hc * 16 + 8], tv[:, hc * 16:hc * 16 + 8], Sv[:, hh, :]), r=["S", "tv"], w=["ti"])
                P.add("dve", lambda e, hc=hc, hh=hh: e.max_index(ti[:, hc * 16 + 8:hc * 16 + 16], tv[:, hc * 16 + 8:hc * 16 + 16], Sv[:, hh, :]), r=["S", "tv"], w=["ti"])
        P.add("dve", lambda e: e.tensor_copy(tif[:], ti[:]), r=["ti"], w=["tif"])
        P.add("dve", lambda e: e.tensor_tensor(candv, tvv[:, :, 0, :].unsqueeze(3).to_broadcast([128, 8, 16, 16]),
                                               tvv[:, :, 1, :].unsqueeze(2).to_broadcast([128, 8, 16, 16]), ALU.add), r=["tv"], w=["cand"])
        for h in range(8):
            P.add("dve", lambda e, h=h: e.max(bs[:, h * 16:h * 16 + 8], candh[:, h, :]), r=["cand"], w=["bs"])
            P.add("dve", lambda e, h=h: e.match_replace(swk[:], bs[:, h * 16:h * 16 + 8], candh[:, h, :], -3e30), r=["cand", "bs"], w=["swk"])
            P.add("dve", lambda e, h=h: e.max(bs[:, h * 16 + 8:h * 16 + 16], swk[:]), r=["swk"], w=["bs"])
            P.add("dve", lambda e, h=h: e.max_index(bci[:, h * 16:h * 16 + 8], bs[:, h * 16:h * 16 + 8], candh[:, h, :]), r=["cand", "bs"], w=["bci"])
            P.add("dve", lambda e, h=h: e.max_index(bci[:, h * 16 + 8:h * 16 + 16], bs[:, h * 16 + 8:h * 16 + 16], candh[:, h, :]), r=["cand", "bs"], w=["bci"])
        P.add("dve", lambda e: e.tensor_copy(bcf[:], bci[:]), r=["bci"], w=["bcf"])
        cq = cand[:].rearrange("p (q j) -> p q j", j=16)
        P.add("dve", lambda e: e.tensor_tensor(cq, bcf[:].unsqueeze(2).to_broadcast([128, 128, 16]),
                                               thr16[:].unsqueeze(1).to_broadcast([128, 128, 16]), ALU.is_ge), r=["bcf", "consts", "bs", "bci"], w=["cand"])
        P.add("dve", lambda e: e.tensor_reduce(av[:], cq, AX.X, ALU.add), r=["cand"], w=["av"])
        P.add("dve", lambda e: e.scalar_tensor_tensor(bv[:], av[:], -16.0, bcf[:], ALU.mult, ALU.add), r=["av", "bcf"], w=["bv"])
        iot_b = iot16[:].unsqueeze(1).unsqueeze(1).to_broadcast([128, 8, 16, 16])
        for (sel_, idxc, dsti, kk) in ((av, 0, i1, "i1"), (bv, 1, i2, "i2")):
            selv = sel_[:].rearrange("p (h k) -> p h k", k=16).unsqueeze(3).to_broadcast([128, 8, 16, 16])
            P.add("dve", lambda e, selv=selv: e.tensor_tensor(candv, selv, iot_b, ALU.is_equal), r=["av", "bv", "consts", "bs", "bci"], w=["cand"])
            P.add("dve", lambda e, idxc=idxc: e.tensor_tensor(candv, candv, tifv[:, :, idxc, :].unsqueeze(2).to_broadcast([128, 8, 16, 16]), ALU.mult),
                  r=["cand", "tif"], w=["cand"])
            P.add("dve", lambda e, dsti=dsti: e.tensor_reduce(dsti[:], cand[:].rearrange("p (q a) -> p q a", a=16), AX.X, ALU.add), r=["cand"], w=[kk])
        P.add("dve", lambda e: e.scalar_tensor_tensor(exf[:], i1[:], 128.0, i2[:], ALU.mult, ALU.add), r=["i1", "i2"], w=["exf"])
        ps_, pk = nps()
        P.add("pe", lambda e, ps_=ps_: e.transpose(ps_[:, 0:128], exf[:], identf[:]), r=["exf", "identf"], w=[pk])
        P.add("dve", lambda e, ps_=ps_: e.tensor_copy(eidx[:], ps_[:, 0:128]), r=[pk], w=["eidx"])
        P.add("dve", lambda e: e.tensor_tensor(wgt[:].rearrange("p (h k) -> p h k", k=16), bsv, bsv[:, :, 0:1].to_broadcast([128, 8, 16]), ALU.subtract), r=["bs"], w=["wgt"])
        P.add("act", lambda e: e.activation(wgt[:], wgt[:], AF.Exp), r=["wgt"], w=["wgt"])
        P.add("dve", lambda e: e.tensor_reduce(ssum[:], wgt[:].rearrange("p (h k) -> p h k", k=16), AX.X, ALU.add), r=["wgt"], w=["ssum"])
        P.add("dve", lambda e: e.reciprocal(ssum[:], ssum[:]), r=["ssum"], w=["ssum"])
        P.add("dve", lambda e: e.tensor_tensor(wgt[:].rearrange("p (h k) -> p h k", k=16), wgt[:].rearrange("p (h k) -> p h k", k=16),
                                               ssum[:].unsqueeze(2).to_broadcast([128, 8, 16]), ALU.mult), r=["wgt", "ssum"], w=["wgt"])
        ps_, pk = nps()
        P.add("pe", lambda e, ps_=ps_: e.transpose(ps_[:, 0:128], wgt[:], identf[:]), r=["wgt", "identf"], w=[pk])
        P.add("act", lambda e, ps_=ps_: e.activation(wT[:], ps_[:, 0:128], AF.Copy), r=[pk], w=["wT"])
        P.add("dve", lambda e: e.memset(actT[:], 0.0), w=["actT"])
        for t in range(n_tok):
            s3, s2 = t % 3, t % 2
            P.add("pool", lambda e, t=t, s3=s3: e.indirect_dma_start(out=ug[s3][:], out_offset=None, in_=pu,
                                                                     in_offset=bass.IndirectOffsetOnAxis(ap=eidx[:, t:t + 1], axis=0)),
                  r=["eidx"], w=["ug%d" % s3], dma="gu%d" % s3)
            P.add("act", lambda e, t=t, s2=s2: e.activation(selb[s2][:], ident[:, t:t + 1].to_broadcast([128, 128]), AF.Copy), r=["ident"], w=["selb%d" % s2])
            for half in range(2):
                P.add("pe", lambda e, half=half, s2=s2: e.matmul(bcp[:, half * 512:(half + 1) * 512], selb[s2][:], hn[:, half * 512:(half + 1) * 512],
                                                                 start=True, stop=True), r=["selb%d" % s2, "hn"], w=["pc"])
            P.add("dve", lambda e, t=t, s3=s3: e.scalar_tensor_tensor(junk[:], ug[s3][:], 1.0, bcp[:, :], ALU.mult, ALU.mult, accum_out=actT[:, t:t + 1]),
                  r=["ug%d" % s3, "pc"], w=["junk", "actT"])
        gelu_tanh(actT[:], gx[:], CT[:], "actT", "gx", "CT")
        P.add("dve", lambda e: e.tensor_tensor(CT[:], CT[:], wT[:], ALU.mult), r=["CT", "wT"], w=["CT"])
        first = True
        for t in range(n_tok):
            s2 = t % 2
            P.add("pool", lambda e, t=t, s2=s2: e.indirect_dma_start(out=vg[s2][:], out_offset=None, in_=pv,
                                                                     in_offset=bass.IndirectOffsetOnAxis(ap=eidx[:, t:t + 1], axis=0)),
                  r=["eidx"], w=["vg%d" % s2], dma="gv%d" % s2)
            P.add("act", lambda e, s2=s2: e.activation(vgb[s2][:], vg[s2][:], AF.Copy), r=["vg%d" % s2], w=["vgb%d" % s2])
            P.add("dve", lambda e, t=t, s2=s2: e.tensor_scalar(Cm[s2][:], Zc[:, 127 - t:255 - t], CT[:, t:t + 1], None, ALU.mult),
                  r=["CT", "consts"], w=["Cm%d" % s2])
            for half in range(2):
                P.add("pe", lambda e, half=half, s2=s2, first=first: e.matmul(pop[:, half * 512:(half + 1) * 512], Cm[s2][:], vgb[s2][:, half * 512:(half + 1) * 512],
                                                                            start=first, stop=True, skip_group_check=True), r=["Cm%d" % s2, "vgb%d" % s2], w=["pd"])
            first = False
        P.add("dve", lambda e: e.tensor_tensor(h1[:], h1[:], pop[:, :], ALU.add), r=["h1", "pd"], w=["h1"])
        rms_tm(C, h1[:].rearrange("p (g d) -> p g d", g=1), hn[:].rearrange("p (g d) -> p g d", g=1), 128, 1, 1024,
               gnb[:].rearrange("p (g d) -> p g d", g=1), "g", r=["h1"], w=["hn"])
        transpose8(hn, hnTv, "hn", "hnT")
        P.add("act", lambda e: e.activation(ptb[:], pt[:], AF.Copy), r=["pt"], w=["ptb"])
        transpose8(ptb, pTv, "ptb", "pT", nch=2)
        for half in range(2):
            ps_, pk = nps()
            for dc in range(8):
                P.add("pe", lambda e, dc=dc, half=half, ps_=ps_: e.matmul(ps_[:, :], hnTv[:, dc, :], gwv[:, dc, half * 512:(half + 1) * 512],
                                                                       start=(dc == 0), stop=(dc == 7)), r=["hnT", "gwb"], w=[pk])
            P.add("act", lambda e, half=half, ps_=ps_: e.activation(gate[:, half * 512:(half + 1) * 512], ps_[:, :], AF.Sigmoid), r=[pk], w=["gate"])
            ps2, pk2 = nps()
            for dc in range(2):
                P.add("pe", lambda e, dc=dc, half=half, ps2=ps2: e.matmul(ps2[:, :], pTv[:, dc, :], plev[:, dc, half * 512:(half + 1) * 512],
                                                                       start=(dc == 0), stop=(dc == 1)), r=["pT", "pleb"], w=[pk2])
            P.add("dve", lambda e, half=half, ps2=ps2: e.tensor_tensor(gate[:, half * 512:(half + 1) * 512], gate[:, half * 512:(half + 1) * 512], ps2[:, :], ALU.mult),
                  r=["gate", pk2], w=["gate"])
        P.add("dve", lambda e: e.tensor_tensor(h1[:], h1[:], gate[:], ALU.add), r=["h1", "gate"], w=["h1"])
        P.add("sp", lambda e, it=it: e.dma_start(out=o_t[it], in_=h1[:]), r=["h1"], w=["out"], dma="out")
    P.emit(final_keys=["out"])
    assert nc.sbuf_bytes_remaining >= 4 * 1024, nc.sbuf_bytes_remaining
    return C


def l2_in_maps(inp, mixed):
    x = np.asarray(inp["x"], np.float32).reshape(16384, 1024)
    p = np.asarray(inp["p"], np.float32)[0].reshape(16384, 256)
    keysT = np.ascontiguousarray(np.asarray(inp["peer_keys"], np.float32)[0].transpose(3, 0, 1, 2).reshape(128, 2048))
    shared = {
        "w_out": np.ascontiguousarray(np.asarray(inp["w_out"], np.float32)[0]),
        "wq": np.ascontiguousarray(np.asarray(inp["peer_wq"], np.float32)[0]),
        "gw": np.ascontiguousarray(np.asarray(inp["ple_gate_w"], np.float32)[0]),
        "ple": np.ascontiguousarray(np.asarray(inp["ple_proj"], np.float32)[0]),
        "keysT": keysT,
        "fn": np.asarray(inp["ffn_norm"], np.float32)[0][None, :].copy(),
        "gn": np.asarray(inp["ple_gate_norm"], np.float32)[0][None, :].copy(),
        "pu": np.ascontiguousarray(np.asarray(inp["peer_u"], np.float32)[0]),
        "pv": np.ascontiguousarray(np.asarray(inp["peer_v"], np.float32)[0]),
    }
    maps = []
    for c in range(8):
        sl = slice(c * 2048, (c + 1) * 2048)
        m = dict(shared)
        m["x"] = np.ascontiguousarray(x[sl])
        m["mixed"] = np.ascontiguousarray(mixed.reshape(16384, 1024)[sl])
        m["p"] = np.ascontiguousarray(p[sl])
        maps.append(m)
    return maps


def assemble_mixed(results, meta):
    mixed = np.zeros((2, T, 1024), np.float32)
    for c in range(8):
        b, u, h0 = meta[c]
        mixed[b, :, u * 128:(u + 1) * 128] = results[c]["o_hg"]
        mixed[b, :, 512 + h0 * 64:512 + h0 * 64 + 128] = results[c]["o_ns"]
    return mixed


def kernel(**inputs):
    C1 = build_l1(16)
    maps1, meta = l1_in_maps(inputs)
    r1 = run_bass_kernel_spmd(C1.nc, maps1, core_ids=list(range(8)))
    mixed = assemble_mixed(r1.results, meta)
    C2 = build_l2(16)
    maps2 = l2_in_maps(inputs, mixed)
    r2 = run_bass_kernel_spmd(C2.nc, maps2, core_ids=list(range(8)))
    out = np.concatenate([r2.results[c]["out"] for c in range(8)], 0).reshape(2, T, 1024)
    return out.astype(np.float32)
```
